# Optimizing a Trainium2 kernel written in Bass

```python
import jax, jax.numpy as jnp
from jax import lax
import numpy as np

D_MODEL = 1024
BATCH = 16
SEQ = 2048
DEPTH = 1
DEC_BATCH = 8
DEC_SEQ = 16
PAST_LEN = 2048

CHUNK = 64
Q_BLOCK = 128
N_MEM = 256
H_FOX = 8
DH_FOX = 64
H_RET = 4
DK_RET = 128
DV_RET = 256
H_MEM = 4
DH_MEM = 128
W_FOX = H_FOX * DH_FOX
W_RET_QK = H_RET * DK_RET
W_RET_V = H_RET * DV_RET
W_MEM = H_MEM * DH_MEM
N_BRANCH = 3
D_IN = 4 * W_FOX + H_FOX + 2 * W_RET_QK + 2 * W_RET_V + 2 * W_MEM + N_BRANCH * D_MODEL
FORGET_BIAS = 3.0
ROPE_BASE = 10000.0
EPS = 1e-6

kernel_name = 'hybrid_fox_retention_memory_stream_step'


def rms_norm(x, g):
    xf = x.astype(jnp.float32)
    y = xf * lax.rsqrt(jnp.mean(xf * xf, axis=-1, keepdims=True) + EPS)
    return (y * g.astype(jnp.float32)).astype(x.dtype)


def head_group_norm(x):
    xf = x.astype(jnp.float32)
    mu = jnp.mean(xf, axis=-1, keepdims=True)
    var = jnp.mean(jnp.square(xf - mu), axis=-1, keepdims=True)
    return (xf - mu) * lax.rsqrt(var + EPS)


def rope(x, pos):
    half = x.shape[-1] // 2
    inv = ROPE_BASE ** (-jnp.arange(half, dtype=jnp.float32) / half)
    ang = pos.astype(jnp.float32)[:, None] * inv[None, :]
    cos = jnp.cos(ang)[None, :, None, :]
    sin = jnp.sin(ang)[None, :, None, :]
    xf = x.astype(jnp.float32)
    x1, x2 = xf[..., :half], xf[..., half:]
    return jnp.concatenate([x1 * cos - x2 * sin, x1 * sin + x2 * cos], axis=-1).astype(x.dtype)


def ret_log_gamma():
    return jnp.log1p(-jnp.exp2(-5.0 - jnp.arange(H_RET, dtype=jnp.float32)))


def split_proj(z):
    sizes = (W_FOX, W_FOX, W_FOX, H_FOX, W_FOX, W_RET_QK, W_RET_QK, W_RET_V, W_RET_V, W_MEM, W_MEM, N_BRANCH * D_MODEL)
    idx = np.cumsum(sizes)[:-1].tolist()
    return jnp.split(z, idx, axis=-1)


def mixer_inputs(x, pos, g_norm, w_in, b_f, b_merge, g_fox_q, g_fox_k, g_mem_q):
    B, T = x.shape[0], x.shape[1]
    h = rms_norm(x, g_norm)
    z = jnp.einsum('btd,de->bte', h, w_in)
    fq, fk, fv, ff, fg, rq, rk, rv, rg, mq, mg, gl = split_proj(z)
    fq = rms_norm(fq.reshape(B, T, H_FOX, DH_FOX), g_fox_q)
    fk = rms_norm(fk.reshape(B, T, H_FOX, DH_FOX), g_fox_k)
    fv = fv.reshape(B, T, H_FOX, DH_FOX)
    logf = jax.nn.log_sigmoid(ff.astype(jnp.float32) + b_f.astype(jnp.float32))
    rq = rope(rq.reshape(B, T, H_RET, DK_RET), pos)
    rk = rope(rk.reshape(B, T, H_RET, DK_RET), pos) * (DK_RET ** -0.5)
    rv = rv.reshape(B, T, H_RET, DV_RET)
    mq = rms_norm(mq.reshape(B, T, H_MEM, DH_MEM), g_mem_q)
    gates = jax.nn.sigmoid((gl + b_merge).astype(jnp.float32)).reshape(B, T, N_BRANCH, D_MODEL)
    return fq, fk, fv, logf, rq, rk, rv, mq, fg, rg, mg, gates


def fox_attend(q, c_q, q_pos, k, v, c_k):
    s = jnp.einsum('bqhd,bkhd->bhqk', q, k, preferred_element_type=jnp.float32) * (DH_FOX ** -0.5)
    s = s + jnp.swapaxes(c_q, 1, 2)[..., :, None] - jnp.swapaxes(c_k, 1, 2)[..., None, :]
    k_pos = jnp.arange(k.shape[1])
    s = jnp.where(k_pos[None, :] <= q_pos[:, None], s, -jnp.inf)
    p = jax.nn.softmax(s, axis=-1).astype(v.dtype)
    return jnp.einsum('bhqk,bkhd->bqhd', p, v)


def fox_prompt(q, k, v, c):
    B, T, H, D = q.shape

    def one_block(bi):
        start = bi * Q_BLOCK
        qb = lax.dynamic_slice_in_dim(q, start, Q_BLOCK, axis=1)
        cb = lax.dynamic_slice_in_dim(c, start, Q_BLOCK, axis=1)
        q_pos = start + jnp.arange(Q_BLOCK)
        return fox_attend(qb, cb, q_pos, k, v, c)

    o = lax.map(one_block, jnp.arange(T // Q_BLOCK))
    return jnp.swapaxes(o, 0, 1).reshape(B, T, H, D)


def retention_block(q, k, v, s, log_g):
    L = q.shape[1]
    i = jnp.arange(L, dtype=jnp.float32)
    dec_q = jnp.exp(log_g[:, None] * (i[None, :] + 1.0))
    diff = i[:, None] - i[None, :]
    dmask = jnp.where(diff >= 0, jnp.exp(log_g[:, None, None] * jnp.maximum(diff, 0.0)), 0.0)
    inter = jnp.einsum('blhk,bhkv->blhv', q, s) * jnp.swapaxes(dec_q, 0, 1)[None, :, :, None]
    scores = jnp.einsum('blhk,bmhk->bhlm', q, k) * dmask[None]
    intra = jnp.einsum('bhlm,bmhv->blhv', scores, v)
    dec_k = jnp.exp(log_g[:, None] * (L - 1.0 - i[None, :]))
    s_new = jnp.exp(log_g * L)[None, :, None, None] * s + jnp.einsum('blhk,blhv,hl->bhkv', k, v, dec_k)
    return inter + intra, s_new


def retention_prompt(q, k, v, log_g):
    B, T, H, dk = q.shape
    dv = v.shape[-1]
    nc = T // CHUNK

    def to_chunks(a):
        return jnp.swapaxes(a.astype(jnp.float32).reshape(B, nc, CHUNK, H, a.shape[-1]), 0, 1)

    def step(s, blk):
        qc, kc, vc = blk
        o, s = retention_block(qc, kc, vc, s, log_g)
        return s, o

    s0 = jnp.zeros((B, H, dk, dv), jnp.float32)
    s_fin, o = lax.scan(step, s0, (to_chunks(q), to_chunks(k), to_chunks(v)))
    return jnp.swapaxes(o, 0, 1).reshape(B, T, H, dv), s_fin


def memory_kv(mem, g_mem_norm, w_mem_kv, g_mem_k):
    B, N = mem.shape[0], mem.shape[1]
    hm = rms_norm(mem, g_mem_norm)
    mk, mv = jnp.split(jnp.einsum('bnd,de->bne', hm, w_mem_kv), 2, axis=-1)
    mk = rms_norm(mk.reshape(B, N, H_MEM, DH_MEM), g_mem_k)
    return mk, mv.reshape(B, N, H_MEM, DH_MEM)


def memory_attend(q, mk, mv):
    s = jnp.einsum('bthd,bmhd->bhtm', q, mk.astype(q.dtype), preferred_element_type=jnp.float32) * (DH_MEM ** -0.5)
    p = jax.nn.softmax(s, axis=-1).astype(q.dtype)
    return jnp.einsum('bhtm,bmhd->bthd', p, mv.astype(q.dtype))


def mixer_output(x, fox_o, fg, ret_o, rg, mem_o, mg, gates, w_p_fox, w_p_ret, w_p_mem, w_out):
    B, T = x.shape[0], x.shape[1]
    a = jnp.einsum('btc,cd->btd', fox_o.reshape(B, T, W_FOX) * jax.nn.silu(fg), w_p_fox)
    r_in = head_group_norm(ret_o).reshape(B, T, W_RET_V).astype(x.dtype) * jax.nn.silu(rg)
    r = jnp.einsum('btc,cd->btd', r_in, w_p_ret)
    m = jnp.einsum('btc,cd->btd', mem_o.reshape(B, T, W_MEM) * jax.nn.silu(mg), w_p_mem)
    merged = (gates[:, :, 0] * a + gates[:, :, 1] * r + gates[:, :, 2] * m).astype(x.dtype)
    return x + jnp.einsum('btd,de->bte', merged, w_out)


def setup_inputs(seed: int = 0) -> dict:
    key = jax.random.key(seed)
    ks = jax.random.split(key, 24)
    f32 = jnp.float32

    def nrm(k, shape, scale):
        return jax.random.normal(k, shape, f32) * scale

    return {
        'x_prompt': nrm(ks[0], (BATCH, SEQ, D_MODEL), 1.0),
        'x_sample': nrm(ks[1], (DEC_BATCH, DEC_SEQ, D_MODEL), 1.0),
        'mem_prompt': nrm(ks[2], (BATCH, N_MEM, D_MODEL), 1.0),
        'cache_fox_k': nrm(ks[3], (DEPTH, DEC_BATCH, PAST_LEN, H_FOX, DH_FOX), 1.0),
        'cache_fox_v': nrm(ks[4], (DEPTH, DEC_BATCH, PAST_LEN, H_FOX, DH_FOX), 1.0),
        'cache_fox_logf': jax.nn.log_sigmoid(FORGET_BIAS + nrm(ks[5], (DEPTH, DEC_BATCH, PAST_LEN, H_FOX), 1.0)),
        'state_ret': nrm(ks[6], (DEPTH, DEC_BATCH, H_RET, DK_RET, DV_RET), 0.5),
        'cache_mem_k': nrm(ks[7], (DEPTH, DEC_BATCH, N_MEM, H_MEM, DH_MEM), 1.0),
        'cache_mem_v': nrm(ks[8], (DEPTH, DEC_BATCH, N_MEM, H_MEM, DH_MEM), 1.0),
        'g_norm': 1.0 + nrm(ks[9], (DEPTH, D_MODEL), 0.02),
        'g_mem_norm': 1.0 + nrm(ks[10], (DEPTH, D_MODEL), 0.02),
        'w_in': nrm(ks[11], (DEPTH, D_MODEL, D_IN), D_MODEL ** -0.5),
        'b_f': FORGET_BIAS + nrm(ks[12], (DEPTH, H_FOX), 0.1),
        'b_merge': nrm(ks[13], (DEPTH, N_BRANCH * D_MODEL), 0.02),
        'g_fox_q': 1.0 + nrm(ks[14], (DEPTH, DH_FOX), 0.02),
        'g_fox_k': 1.0 + nrm(ks[15], (DEPTH, DH_FOX), 0.02),
        'g_mem_q': 1.0 + nrm(ks[16], (DEPTH, DH_MEM), 0.02),
        'g_mem_k': 1.0 + nrm(ks[17], (DEPTH, DH_MEM), 0.02),
        'w_mem_kv': nrm(ks[18], (DEPTH, D_MODEL, 2 * W_MEM), D_MODEL ** -0.5),
        'w_p_fox': nrm(ks[19], (DEPTH, W_FOX, D_MODEL), W_FOX ** -0.5),
        'w_p_ret': nrm(ks[20], (DEPTH, W_RET_V, D_MODEL), W_RET_V ** -0.5),
        'w_p_mem': nrm(ks[21], (DEPTH, W_MEM, D_MODEL), W_MEM ** -0.5),
        'w_out': nrm(ks[22], (DEPTH, D_MODEL, D_MODEL), D_MODEL ** -0.5),
    }


def reference(x_prompt, x_sample, mem_prompt, cache_fox_k, cache_fox_v, cache_fox_logf, state_ret, cache_mem_k, cache_mem_v,
              g_norm, g_mem_norm, w_in, b_f, b_merge, g_fox_q, g_fox_k, g_mem_q, g_mem_k, w_mem_kv,
              w_p_fox, w_p_ret, w_p_mem, w_out):
    log_g = ret_log_gamma()
    t_p = x_prompt.shape[1]
    t_s = x_sample.shape[1]
    past = cache_fox_k.shape[2]
    pos_p = jnp.arange(t_p, dtype=jnp.int32)
    pos_s = past + jnp.arange(t_s, dtype=jnp.int32)
    y_p, y_s = x_prompt, x_sample
    fk_p, fv_p, lf_p, sr_p, mk_p, mv_p = [], [], [], [], [], []
    fk_s, fv_s, lf_s, sr_s = [], [], [], []
    for l in range(DEPTH):
        w_mix = (g_norm[l], w_in[l], b_f[l], b_merge[l], g_fox_q[l], g_fox_k[l], g_mem_q[l])
        w_outs = (w_p_fox[l], w_p_ret[l], w_p_mem[l], w_out[l])
        fq, fk, fv, logf, rq, rk, rv, mq, fg, rg, mg, gates = mixer_inputs(y_p, pos_p, *w_mix)
        c = jnp.cumsum(logf, axis=1)
        fox_o = fox_prompt(fq, fk, fv, c)
        ret_o, s_fin = retention_prompt(rq, rk, rv, log_g)
        mk, mv = memory_kv(mem_prompt, g_mem_norm[l], w_mem_kv[l], g_mem_k[l])
        mem_o = memory_attend(mq, mk, mv)
        y_p = mixer_output(y_p, fox_o, fg, ret_o, rg, mem_o, mg, gates, *w_outs)
        fk_p.append(fk)
        fv_p.append(fv)
        lf_p.append(logf)
        sr_p.append(s_fin.astype(x_prompt.dtype))
        mk_p.append(mk)
        mv_p.append(mv)
        fq, fk, fv, logf, rq, rk, rv, mq, fg, rg, mg, gates = mixer_inputs(y_s, pos_s, *w_mix)
        k_all = jnp.concatenate([cache_fox_k[l].astype(fk.dtype), fk], axis=1)
        v_all = jnp.concatenate([cache_fox_v[l].astype(fv.dtype), fv], axis=1)
        c_all = jnp.cumsum(jnp.concatenate([cache_fox_logf[l].astype(jnp.float32), logf], axis=1), axis=1)
        fox_o = fox_attend(fq, c_all[:, past:], pos_s, k_all, v_all, c_all)
        ret_o, s_new = retention_block(rq.astype(jnp.float32), rk.astype(jnp.float32), rv.astype(jnp.float32),
                                       state_ret[l].astype(jnp.float32), log_g)
        mem_o = memory_attend(mq, cache_mem_k[l], cache_mem_v[l])
        y_s = mixer_output(y_s, fox_o, fg, ret_o, rg, mem_o, mg, gates, *w_outs)
        fk_s.append(fk)
        fv_s.append(fv)
        lf_s.append(logf)
        sr_s.append(s_new.astype(x_sample.dtype))
    return (y_p, y_s,
            jnp.stack(fk_p, 0), jnp.stack(fv_p, 0), jnp.stack(lf_p, 0), jnp.stack(sr_p, 0), jnp.stack(mk_p, 0), jnp.stack(mv_p, 0),
            jnp.stack(fk_s, 0), jnp.stack(fv_s, 0), jnp.stack(lf_s, 0), jnp.stack(sr_s, 0))
```

```python
import contextlib
import numpy as np
import concourse.bass as bass
import concourse.mybir as mybir
from concourse.bass_utils import run_bass_kernel_spmd

F32 = mybir.dt.float32
BF16 = mybir.dt.bfloat16
ALU = mybir.AluOpType
AF = mybir.ActivationFunctionType
AX = mybir.AxisListType

ENGS = ("pe", "act", "dve", "pool", "sp")
SAME_ENG_GAP = 10 ** 9
NSEM = {"ld": 8, "st": 8, "wl": 4, "cv": 4, "ws": 4}

D = 1024
SEQ = 2048
NCORES = 8
EPS = 1e-6
D_IN = 9224
OFF = dict(fq=0, fk=512, fv=1024, ff=1536, fg=1544, rq=2056, rk=2568, rv=3080, rg=4104,
           mq=5128, mg=5640, gl=6152)
TP = 512


class Op:
    __slots__ = ("eng", "fn", "deps", "idx", "dma", "stream", "dma_idx", "signal", "count")


class Prog:
    def __init__(self, nc):
        self.nc = nc
        self.ops = {e: [] for e in ENGS}
        self.lastw = {}
        self.readers = {}
        self.streams = {}

    @staticmethod
    def _dk(op):
        return ("dma", op.stream, op.dma_idx) if op.dma else ("eng", op.eng)

    def add(self, eng, fn, reads=(), writes=(), dma=False, stream=None):
        op = Op()
        op.eng = eng
        op.fn = fn
        op.dma = dma
        op.stream = stream
        op.signal = False
        op.count = 0
        op.dma_idx = 0
        if dma:
            lst = self.streams.setdefault(stream, [])
            op.dma_idx = len(lst)
            lst.append(op)
        deps = {}

        def put(d):
            k = self._dk(d)
            o = deps.get(k)
            if o is None or (d.dma_idx if d.dma else d.idx) > (o.dma_idx if o.dma else o.idx):
                deps[k] = d

        for k in reads:
            w = self.lastw.get(k)
            if w is not None:
                put(w)
        for k in writes:
            w = self.lastw.get(k)
            if w is not None:
                put(w)
            for r in self.readers.get(k, {}).values():
                put(r)
        op.deps = deps
        op.idx = len(self.ops[eng])
        self.ops[eng].append(op)
        mk = self._dk(op)
        for k in reads:
            self.readers.setdefault(k, {})[mk] = op
        for k in writes:
            self.lastw[k] = op
            self.readers[k] = {}
        return op

    def barrier(self, engs=ENGS):
        lasts = []
        for e in ENGS:
            for o in reversed(self.ops[e]):
                if not o.dma and o.fn is not None:
                    lasts.append(o)
                    break
        for s, lst in self.streams.items():
            lasts.extend(lst[-NSEM[s]:])
        for e in engs:
            op = self.add(e, None)
            for d in lasts:
                if d is not op:
                    op.deps[self._dk(d)] = d

    def _needs_wait(self, op, d):
        if d.dma:
            return True
        if d.eng == op.eng and not op.dma:
            if op.eng == "pe":
                return False
            if op.idx - d.idx > SAME_ENG_GAP:
                return False
        return True

    def emit(self):
        nc = self.nc
        for e in ENGS:
            for op in self.ops[e]:
                for d in op.deps.values():
                    if not d.dma and self._needs_wait(op, d):
                        d.signal = True
        for e in ENGS:
            c = 0
            for op in self.ops[e]:
                if op.dma:
                    continue
                if op.signal and op.fn is not None:
                    c += 1
                    op.count = c
                else:
                    op.signal = False
            assert c < 60000, (e, c)
        with contextlib.ExitStack() as st:
            esem = {e: st.enter_context(nc.semaphore("s_" + e)) for e in ENGS}
            dsem = {(s, j): st.enter_context(nc.semaphore("d_%s%d" % (s, j)))
                    for s in self.streams for j in range(NSEM[s])}
            block = st.enter_context(nc.Block())

            def run(e, engine):
                waited = {}
                for op in self.ops[e]:
                    for d in op.deps.values():
                        if not self._needs_wait(op, d):
                            continue
                        if d.dma:
                            nsem = NSEM[d.stream]
                            sem = dsem[(d.stream, d.dma_idx % nsem)]
                            val = 16 * (d.dma_idx // nsem + 1)
                            key = ("d", d.stream, d.dma_idx % nsem)
                        else:
                            if d.fn is None:
                                continue
                            assert d.signal
                            sem = esem[d.eng]
                            val = d.count
                            key = ("e", d.eng)
                        if waited.get(key, 0) >= val:
                            continue
                        engine.wait_ge(sem, val)
                        waited[key] = val
                    if op.dma:
                        nsem = NSEM[op.stream]
                        j = op.dma_idx % nsem
                        val = 16 * (op.dma_idx // nsem)
                        if val > 0 and waited.get(("d", op.stream, j), 0) < val:
                            engine.wait_ge(dsem[(op.stream, j)], val)
                            waited[("d", op.stream, j)] = val
                    if op.fn is not None:
                        ins = op.fn(engine)
                        if op.dma:
                            ins.then_inc(dsem[(op.stream, op.dma_idx % NSEM[op.stream])], 16)
                        elif op.signal:
                            ins.then_inc(esem[e], 1)

            @block.tensor
            def _(eng):
                run("pe", eng)

            @block.scalar
            def _(eng):
                run("act", eng)

            @block.vector
            def _(eng):
                run("dve", eng)

            @block.gpsimd
            def _(eng):
                run("pool", eng)

            @block.sync
            def _(eng):
                run("sp", eng)


class Rot:
    def __init__(self, st, nc, name, n, shape, dt):
        self.tiles = [st.enter_context(nc.sbuf_tensor("%s%d" % (name, i), list(shape), dt)) for i in range(n)]
        self.name = name
        self.i = 0

    def next(self):
        j = self.i % len(self.tiles)
        self.i += 1
        return self.tiles[j], "%s%d" % (self.name, j)


def build_program(tp=TP):
    nc = bass.Bass("TRN2", target_bir_lowering=False)
    P = Prog(nc)

    def din(name, shape):
        return nc.dram_tensor(name, list(shape), F32, kind="ExternalInput").ap()

    def dout(name, shape):
        return nc.dram_tensor(name, list(shape), F32, kind="ExternalOutput").ap()

    x_p = din("x_p", [2, SEQ, D])
    x_s = din("x_s", [16, D])
    mem_p = din("mem_p", [2, 256, D])
    ck = din("ck", [SEQ, 512])
    cv = din("cv", [SEQ, 512])
    clf = din("clf", [SEQ, 8])
    sret = din("sret", [4, 128, 256])
    cmk = din("cmk", [256, 512])
    cmv = din("cmv", [256, 512])
    g_norm = din("g_norm", [D])
    g_mem_norm = din("g_mem_norm", [D])
    w_in = din("w_in", [D, D_IN])
    b_f = din("b_f", [8])
    b_merge = din("b_merge", [3 * D])
    g_fox_q = din("g_fox_q", [64])
    g_fox_k = din("g_fox_k", [64])
    g_mem_q = din("g_mem_q", [128])
    g_mem_k = din("g_mem_k", [128])
    w_mem_kv = din("w_mem_kv", [D, D])
    w_p_fox = din("w_p_fox", [512, D])
    w_p_ret = din("w_p_ret", [D, D])
    w_p_mem = din("w_p_mem", [512, D])
    w_out = din("w_out", [D, D])
    c_ident = din("c_ident", [128, 128])
    c_triu = din("c_triu", [128, 128])
    c_maskneg = din("c_maskneg", [128, 128])
    c_mask01 = din("c_mask01", [128, 128])
    rope_p = din("rope_p", [SEQ, 1024])
    rope_s = din("rope_s", [128, 1024])

    y_p = dout("y_p", [2, SEQ, D])
    y_s = dout("y_s", [16, D])
    fk_p = dout("fk_p", [2, SEQ, 512])
    fv_p = dout("fv_p", [2, SEQ, 512])
    lf_p = dout("lf_p", [2, SEQ, 8])
    sr_p = dout("sr_p", [2, 4, 128, 256])
    mk_p = dout("mk_p", [2, 256, 512])
    mv_p = dout("mv_p", [2, 256, 512])
    fk_s = dout("fk_s", [16, 512])
    fv_s = dout("fv_s", [16, 512])
    lf_s = dout("lf_s", [16, 8])
    sr_s = dout("sr_s", [4, 128, 256])

    WNAMES = ["fq", "fk", "fv", "fg", "rq", "rk", "rv0", "rv1", "rg0", "rg1", "mq", "mg",
              "g00", "pf0", "g10", "pr0", "g20", "pm0", "g01", "pf1", "g11", "pr1", "g21", "pm1", "wo0", "wo1", "mk", "mv"]
    wsc = nc.dram_tensor("wsc", [len(WNAMES), 128, 4096], BF16, kind="Internal").ap()
    NSP = tp // 128
    with contextlib.ExitStack() as st:
        def sb(name, shape, dt):
            return st.enter_context(nc.sbuf_tensor(name, list(shape), dt))

        ident_f = sb("ident_f", [128, 128], F32)
        triu_f = sb("triu_f", [128, 128], F32)
        ones_f = sb("ones_f", [128, 128], F32)
        ident_bf = sb("ident_bf", [128, 128], BF16)
        ones_bf = sb("ones_bf", [128, 128], BF16)
        maskneg_bf = sb("maskneg_bf", [128, 128], BF16)
        mask01_f = sb("mask01_f", [128, 128], F32)
        mhalf = sb("mhalf", [128, 32], F32)
        sqjunk = sb("sqjunk", [128, D], mybir.dt.float8e4)
        mone = sb("mone", [128, 8], F32)
        epst = sb("epst", [128, 8], F32)
        gb = sb("gb", [128, D], F32)
        gk_b = sb("gk_b", [128, 64], F32)
        gmk_b = sb("gmk_b", [128, 128], F32)
        bf_b = sb("bf_b", [128, 8], F32)
        gq67 = sb("gq67", [128, 1], F32)
        gmq_s = sb("gmq_s", [128, 1], F32)
        bm_half = sb("bm_half", [128, 24], F32)
        wff = sb("wff", [128, 8, 8], BF16)
        kT_aug = sb("kT_aug", [128, 8, 17 * 128], BF16)
        Vt = sb("Vt", [128, 17, 768], BF16)
        negc = sb("negc", [128, 17, 8], F32)
        mkT = sb("mkT", [128, 4, 256], BF16)
        mvt = sb("mvt", [128, 2, 512], BF16)
        S_f = sb("S_f", [128, 4, 256], F32)
        S_bf = sb("S_bf", [128, 4, 256], BF16)
        carry = sb("carry", [128, 8], F32)
        hT = sb("hT", [128, 8, tp], BF16)
        qT_aug = sb("qT_aug", [128, 8, tp], BF16)
        foxg = sb("foxg", [128, 4, tp], BF16)
        rqT = sb("rqT", [128, 4, tp], BF16)
        rkT = sb("rkT", [128, 4, tp], BF16)
        rk_tok = sb("rk_tok", [128, NSP, 512], BF16)
        rvt = sb("rvt", [128, NSP, 1024], BF16)
        retg = sb("retg", [128, 8, tp], BF16)
        mqT = sb("mqT", [128, 4, tp], BF16)
        memg = sb("memg", [128, 4, tp], BF16)
        mrgT = qT_aug
        rtbuf = sb("rtbuf", [128, 2048], F32)
        acc = rtbuf[:, :].rearrange("p (c t) -> p c t", c=4)
        logf = sb("logf", [128, 17, 8], F32)
        cc_ = sb("cc_", [128, NSP, 8], F32)
        cparts = sb("cparts", [128, NSP, 8, 3], BF16)
        ss = sb("ss", [128, 8], F32)
        sqj = rtbuf[:, 0:1024]
        wslot = Rot(st, nc, "ws", 4, [128, 4096], BF16)
        xt = Rot(st, nc, "xt", 2, [128, D], F32)
        xs = Rot(st, nc, "xs", 2, [128, D], BF16)
        rt_i = [0]

        def rt_next():
            j = rt_i[0] % 2
            rt_i[0] += 1
            return rtbuf[:, j * 1024:(j + 1) * 1024], "rt%d" % j
        f32w = Rot(st, nc, "fw", 4, [128, 512], F32)
        bfw = Rot(st, nc, "bw", 4, [128, 512], BF16)
        qat = Rot(st, nc, "qat", 2, [128, 8, 72], BF16)
        kat = Rot(st, nc, "kat", 2, [128, 8, 72], BF16)
        sm = Rot(st, nc, "sm", 8, [128, 32], F32)
        sm2 = Rot(st, nc, "sn", 64, [128, 8], F32)
        ps_z = [st.enter_context(nc.psum_tensor("psz%d" % i, [128, 512], F32)) for i in range(2)]
        ps_t = [st.enter_context(nc.psum_tensor("pst%d" % i, [128, 1024], BF16)) for i in range(2)]
        ps_s = [st.enter_context(nc.psum_tensor("pss%d" % i, [128, 512], F32)) for i in range(2)]
        ps_o = [st.enter_context(nc.psum_tensor("pso%d" % i, [128, 512], F32)) for i in range(2)]
        cnt = dict(z=0, t=0, s=0, o=0)

        ps_zs = ps_z + ps_s

        def psum(kind):
            if kind in ("z", "s"):
                j = cnt["z"] % 4
                cnt["z"] += 1
                return ps_zs[j], "pszs%d" % j
            arr = dict(t=ps_t, o=ps_o)[kind]
            j = cnt[kind] % 2
            cnt[kind] += 1
            return arr[j], "ps%s%d" % (kind, j)

        def mm(out, lhsT, rhs, start, stop, R, W):
            P.add("pe", lambda e: e.matmul(out, lhsT=lhsT, rhs=rhs, start=start, stop=stop), R, W)

        def tp_(out, in_, R, W):
            P.add("pe", lambda e: e.transpose(out=out, in_=in_, identity=ident_bf[:]), R, W)

        def act(out, in_, func, R, W, bias=None, scale=1.0, accum=None):
            kw = dict(scale=scale)
            if bias is not None:
                kw["bias"] = bias
            if accum is not None:
                kw["accum_out"] = accum
            P.add("act", lambda e: e.activation(out=out, in_=in_, func=func, **kw), R, W)

        def tt(eng, out, in0, in1, op, R, W):
            P.add(eng, lambda e: e.tensor_tensor(out=out, in0=in0, in1=in1, op=op), R, W)

        def ts(eng, out, in0, s1, s2, op0, op1, R, W):
            if s2 is None:
                P.add(eng, lambda e: e.tensor_scalar(out=out, in0=in0, scalar1=s1, scalar2=None, op0=op0), R, W)
            else:
                P.add(eng, lambda e: e.tensor_scalar(out=out, in0=in0, scalar1=s1, scalar2=s2, op0=op0, op1=op1), R, W)

        def stt(eng, out, in0, scalar, in1, op0, op1, R, W):
            P.add(eng, lambda e: e.scalar_tensor_tensor(out=out, in0=in0, scalar=scalar, in1=in1, op0=op0, op1=op1), R, W)

        def cp(eng, out, in_, R, W):
            if eng == "act":
                P.add("act", lambda e: e.copy(out=out, in_=in_), R, W)
            else:
                P.add(eng, lambda e: e.tensor_copy(out=out, in_=in_), R, W)

        def memset(eng, ap, val, W):
            P.add(eng, lambda e: e.memset(ap, val), (), W)

        def dma(q, out, in_, R, W, stream, slow=False):
            if slow:
                P.add(q, lambda e: e.dma_start(out=out, in_=in_, allow_slow_non_contiguous=True), R, W, dma=True, stream=stream)
            else:
                P.add(q, lambda e: e.dma_start(out=out, in_=in_), R, W, dma=True, stream=stream)

        def rstd_from(ssum_ap, n, scale, R_keys):
            v, vk = sm2.next()
            ts("dve", v[:, 0:n], ssum_ap, scale, EPS, ALU.mult, ALU.add, R_keys, [vk])
            r, rk = sm2.next()
            tt("pool", r[:, 0:n], v[:, 0:n], mhalf[:, 0:n], ALU.pow, [vk], [rk])
            return r, rk

        dma("sp", ident_f[:], c_ident, [], ["c0"], "ld")
        dma("sp", triu_f[:], c_triu, [], ["c1"], "ld")
        dma("sp", mask01_f[:], c_mask01, [], ["c2"], "ld")
        tmpm, tmpk = f32w.next()
        dma("sp", tmpm[:, 0:128], c_maskneg, [], [tmpk], "ld")
        dma("sp", gk_b[:], g_fox_k.partition_broadcast(128), [], ["c3"], "ld")
        dma("sp", gmk_b[:], g_mem_k.partition_broadcast(128), [], ["c4"], "ld")
        dma("sp", bf_b[:], b_f.partition_broadcast(128), [], ["c5"], "ld")
        memset("dve", gq67[:], 1.0, ["c6"])
        dma("sp", gq67[0:64, :], g_fox_q.rearrange("(p o) -> p o", o=1), ["c6"], ["c6"], "ld")
        dma("sp", gmq_s[:], g_mem_q.rearrange("(p o) -> p o", o=1), [], ["c7"], "ld")
        dma("sp", bm_half[:], b_merge.rearrange("(c p) -> p c", p=128), [], ["c8"], "ld", slow=True)
        dma("pool", wff[:], w_in[:, OFF["ff"]:OFF["ff"] + 8].rearrange("(c p) n -> p c n", p=128), [], ["c9"], "cv")
        cp("dve", ident_bf[:], ident_f[:], ["c0"], ["c10"])
        cp("dve", maskneg_bf[:], tmpm[:, 0:128], [tmpk], ["c11"])
        memset("dve", ones_f[:], 1.0, ["c12"])
        memset("dve", ones_bf[:], 1.0, ["c13"])
        memset("dve", mhalf[:], -0.5, ["c14"])
        memset("dve", mone[:], -1.0, ["c17"])
        memset("dve", epst[:], EPS, ["c18"])
        ts("dve", gq67[0:64, :], gq67[0:64, :], 0.125, None, ALU.mult, None, ["c6"], ["c6"])
        ts("dve", gmq_s[:], gmq_s[:], float(128 ** -0.5), None, ALU.mult, None, ["c7"], ["c7"])
        ts("dve", bm_half[:], bm_half[:], 0.5, None, ALU.mult, None, ["c8"], ["c8"])
        memset("dve", kT_aug[:], 1.0, ["c15"])
        memset("pool", Vt[:], 1.0, ["c16"])
        for t_, k_ in zip(qat.tiles + kat.tiles, ["qat0", "qat1", "kat0", "kat1"]):
            memset("dve", t_[:], 1.0, [k_])
        P.barrier()

        plan = []
        wpos = [0]

        def w8(mat, col0):
            return ("k8", mat[:, col0:col0 + 512].rearrange("(c p) n -> p c n", p=128))

        def w4(mat, col0):
            return ("k4", mat[:, col0:col0 + 512].rearrange("(c p) n -> p c n", p=128))

        def plan_pass():
            sp = []
            for nme in ("fq", "fk", "fv", "fg", "rq", "rk"):
                sp.append((nme, w8(w_in, OFF[nme])))
            sp.append(("rv0", w8(w_in, OFF["rv"])))
            sp.append(("rv1", w8(w_in, OFF["rv"] + 512)))
            sp.append(("rg0", w8(w_in, OFF["rg"])))
            sp.append(("rg1", w8(w_in, OFF["rg"] + 512)))
            sp.append(("mq", w8(w_in, OFF["mq"])))
            sp.append(("mg", w8(w_in, OFF["mg"])))
            for j in range(2):
                sp.append(("g0%d" % j, w8(w_in, OFF["gl"] + j * 512)))
                sp.append(("pf%d" % j, w4(w_p_fox, j * 512)))
                sp.append(("g1%d" % j, w8(w_in, OFF["gl"] + 1024 + j * 512)))
                sp.append(("pr%d" % j, w8(w_p_ret, j * 512)))
                sp.append(("g2%d" % j, w8(w_in, OFF["gl"] + 2048 + j * 512)))
                sp.append(("pm%d" % j, w4(w_p_mem, j * 512)))
            sp.append(("wo0", w8(w_out, 0)))
            sp.append(("wo1", w8(w_out, 512)))
            return sp

        passes = []
        for b in range(2):
            for p in range(SEQ // tp):
                passes.append(("p", b, p))
        passes.append(("s", 0, 0))
        for kind, b, p in passes:
            if kind == "p" and p == 0:
                plan.append(("mk", w8(w_mem_kv, 0)))
                plan.append(("mv", w8(w_mem_kv, 512)))
            plan.extend(plan_pass())
        loaded = {}
        issued = [0]
        wspec = {}
        for nme, spec in plan:
            if nme not in wspec:
                wspec[nme] = spec
        seen = set()

        def w_issue(upto):
            while issued[0] < min(upto, len(plan)):
                i = issued[0]
                nme, (shp, src) = plan[i]
                slot, skey = wslot.next()
                ti = WNAMES.index(nme)
                ncol = 4096 if shp == "k8" else 2048
                dst = slot[:, 0:ncol].rearrange("p (c n) -> p c n", c=ncol // 512)
                if nme not in seen:
                    seen.add(nme)
                    dma("pool", dst, src, [], [skey], "cv")
                    dma("sp", wsc[ti, :, 0:ncol], slot[:, 0:ncol], [skey], ["wsc_" + nme], "ws")
                else:
                    dma("sp", slot[:, 0:ncol], wsc[ti, :, 0:ncol], ["wsc_" + nme], [skey], "wl")
                loaded[i] = (dst, skey)
                issued[0] += 1

        def wget(nme):
            i = wpos[0]
            assert plan[i][0] == nme, (plan[i][0], nme)
            w_issue(i + 3)
            wpos[0] += 1
            return loaded[i]

        active = []

        def step():
            for g in list(active):
                try:
                    next(g)
                except StopIteration:
                    active.remove(g)

        def pipe(gen):
            active.insert(0, gen)
            step()

        def flush():
            while active:
                step()

        def one(fn):
            def g():
                fn()
                return
                yield
            return g()

        def x_items(src_fn, ns, nrows, col0, gap):
            def item(s):
                gap_s = gap[s] if isinstance(gap, (list, tuple)) else gap
                x, xk = xt.next()
                if nrows < 128:
                    memset("dve", x[:], 0.0, [xk])
                dma("sp", x[0:nrows, :], src_fn(s), [], [xk], "ld")
                P.add("act", lambda e: e.activation(out=sqjunk[:], in_=x[:], func=AF.Square, accum_out=ss[:, s:s + 1],
                                                   saturate=False), [xk], ["ss%d" % s, "sqjunk"])
                r, rk = rstd_from(ss[:, s:s + 1], 1, 1.0 / D, ["ss%d" % s])
                for _ in range(gap_s):
                    yield
                xb, xbk = xs.next()
                stt("dve", xb[:], x[:], r[:, 0:1], gb[:], ALU.mult, ALU.mult, [xk, rk, "gb"], [xbk])
                for _ in range(gap_s):
                    yield
                pt, ptk = psum("t")
                for k in range(8):
                    tp_(pt[:, k * 128:(k + 1) * 128], xb[:, k * 128:(k + 1) * 128], [xbk], [ptk])
                cp("act", hT[:, :, col0 + s * 128: col0 + (s + 1) * 128],
                   pt[:, :].rearrange("p (k t) -> p k t", k=8), [ptk], ["hT%d" % (col0 // 128 + s)])
            return [item(s) for s in range(ns)]

        def xphase(src_fn, ns, nrows, col0):
            for g in x_items(src_fn, ns, nrows, col0, 1):
                pipe(g)
            flush()

        def hkeys(c0, n):
            return ["hT%d" % i for i in range(c0 // 128, (c0 + n) // 128)]

        def tok_section(wt, wk, s, ncols=512):
            z, zk = psum("z")
            for k in range(8):
                mm(z[:, 0:ncols], hT[:, k, s * 128:(s + 1) * 128], wt[:, k, 0:ncols], k == 0, k == 7,
                   ["hT%d" % s, wk], [zk])
            return z, zk

        def feat_gate(wt, wk, cc, c0, n, dst, dkey):
            g, gk = psum("z")
            for k in range(8):
                mm(g[:, 0:n], wt[:, k, cc * 128:(cc + 1) * 128], hT[:, k, c0:c0 + n], k == 0, k == 7,
                   hkeys(c0, n) + [wk], [gk])
            th, thk = bfw.next()
            act(th[:, 0:n], g[:, 0:n], AF.Tanh, [gk], [thk], scale=0.5)
            stt("dve", dst, th[:, 0:n], 1.0, g[:, 0:n], ALU.add, ALU.mult, [thk, gk], [dkey])

        def head_rstd(z, zk, nh, dh):
            sq, sqk = f32w.next()
            act(sq[:, 0:nh * dh], z[:, 0:nh * dh], AF.Square, [zk], [sqk])
            s_, sk_ = sm2.next()
            P.add("dve", lambda e: e.tensor_reduce(out=s_[:, 0:nh], in_=sq[:, 0:nh * dh].rearrange("p (h d) -> p h d", h=nh),
                                                   axis=AX.X, op=ALU.add), [sqk], [sk_])
            return rstd_from(s_[:, 0:nh], nh, 1.0 / dh, [sk_])

        def k_side(kf_ap, kfk, kb):
            ka, kak = kat.next()
            cp("act", ka[:, :, 0:64], kf_ap.rearrange("p (h d) -> p h d", h=8), [kfk], [kak])
            yield
            yield
            pt, ptk = psum("t")
            for h in range(8):
                tp_(pt[0:67, h * 128:(h + 1) * 128], ka[:, h, 0:67], [kak], [ptk])
            cp("dve", kT_aug[0:67, :, kb * 128:(kb + 1) * 128],
               pt[0:67, :].rearrange("p (h t) -> p h t", h=8), [ptk], ["kT%d" % kb])

        def v_side(v_ap, vk, kb):
            dstv = Vt[:, kb, :].rearrange("p (a c d) -> p a c d", a=4, c=3)[:, :, 0:3:2, :]
            cp("dve", dstv, v_ap.rearrange("p (a c d) -> p a c d", a=4, c=2), [vk], ["V%d" % kb])

        def cumsum_block(kb, lfslot):
            pz, pzk = psum("z")
            mm(pz[:, 0:8], triu_f[:], logf[:, lfslot, :], True, True, ["lf%d" % lfslot], [pzk])
            mm(pz[:, 8:16], ones_f[:], logf[:, lfslot, :], True, True, ["lf%d" % lfslot], [pzk])
            c, ckey = sm2.next()
            tt("dve", c[:, 0:8], pz[:, 0:8], carry[:], ALU.add, [pzk, "carry"], [ckey])
            tt("dve", carry[:], pz[:, 8:16], carry[:], ALU.add, [pzk, "carry", ckey], ["carry"])
            ts("dve", negc[:, kb, :], c[:, 0:8], -1.0, None, ALU.mult, None, [ckey], ["negc%d" % kb])
            return c, ckey

        SFK = ["S_f%d" % i for i in range(4)]
        SBK = ["S_bf%d" % i for i in range(4)]

        def run_pass(kind, b, p, skip_x, prestage_next):
            sample = kind == "s"
            ns = 1 if sample else NSP
            n = ns * 128
            nrows = 16 if sample else 128
            kb0 = 16 if sample else p * NSP
            Ls = 16 if sample else 128
            gL = [float((1.0 - 2.0 ** (-5 - h)) ** Ls) for h in range(4)]

            def rows(ap2d, s):
                if sample:
                    return ap2d[0:16, :]
                t0 = p * tp + s * 128
                return ap2d[b, t0:t0 + 128, :]

            if sample or p == 0:
                memset("dve", carry[:], 0.0, ["carry"])
            if sample:
                dma("sp", S_f[:], sret.rearrange("h k v -> k h v"), [], SFK, "ld")
                cp("dve", S_bf[:], S_f[:], SFK, SBK)
                def cmk_item(s):
                    kf, kfk = f32w.next()
                    dma("sp", kf[:], cmk[s * 128:(s + 1) * 128, :], [], [kfk], "ld")
                    mkt_, mktk = bfw.next()
                    cp("act", mkt_[:], kf[:], [kfk], [mktk])
                    yield
                    pt, ptk = psum("t")
                    for h in range(4):
                        tp_(pt[:, h * 128:(h + 1) * 128], mkt_[:, h * 128:(h + 1) * 128], [mktk], [ptk])
                    cp("dve", mkT[:, :, s * 128:(s + 1) * 128], pt[:, 0:512].rearrange("p (h t) -> p h t", h=4), [ptk], ["mkT"])
                for s in range(2):
                    pipe(cmk_item(s))
                    vf, vfk = f32w.next()
                    dma("sp", vf[:], cmv[s * 128:(s + 1) * 128, :], [], [vfk], "ld")
                    cp("act", mvt[:, s, :], vf[:], [vfk], ["mvt"])
                flush()
                dma("sp", logf[:, 0:16, :], clf.rearrange("(k p) h -> p k h", p=128), [], ["lf%d" % i for i in range(16)], "ld")
                for kb in range(16):
                    kf, kfk = f32w.next()
                    dma("sp", kf[:], ck[kb * 128:(kb + 1) * 128, :], [], [kfk], "ld")
                    pipe(k_side(kf[:], kfk, kb))
                    vf, vfk = f32w.next()
                    dma("sp", vf[:], cv[kb * 128:(kb + 1) * 128, :], [], [vfk], "ld")
                    v_side(vf[:], vfk, kb)
                    cumsum_block(kb, kb)
            elif p == 0:
                memset("dve", S_f[:], 0.0, SFK)
                memset("dve", S_bf[:], 0.0, SBK)
                dma("sp", gb[:], g_mem_norm.partition_broadcast(128), [], ["gb"], "ld")
                xphase(lambda s: mem_p[b, s * 128:(s + 1) * 128, :], 2, 128, 0)
                wt, wk = wget("mk")

                def mk_item(s):
                    z, zk = tok_section(wt, wk, s)
                    r, rk = head_rstd(z, zk, 4, 128)
                    yield
                    kn, knk = f32w.next()
                    tt("dve", kn[:].rearrange("p (h d) -> p h d", h=4), z[:, :].rearrange("p (h d) -> p h d", h=4),
                       r[:, 0:4].unsqueeze(2).broadcast_to([128, 4, 128]), ALU.mult, [zk, rk], [knk])
                    ko, kok = f32w.next()
                    tt("pool", ko[:].rearrange("p (h d) -> p h d", h=4), kn[:].rearrange("p (h d) -> p h d", h=4),
                       gmk_b[:, :].unsqueeze(1).broadcast_to([128, 4, 128]), ALU.mult, [knk], [kok])
                    dma("pool", mk_p[b, s * 128:(s + 1) * 128, :], ko[:], [kok], [], "st")
                    mkt_, mktk = bfw.next()
                    cp("act", mkt_[:], ko[:], [kok], [mktk])
                    yield
                    pt, ptk = psum("t")
                    for h in range(4):
                        tp_(pt[:, h * 128:(h + 1) * 128], mkt_[:, h * 128:(h + 1) * 128], [mktk], [ptk])
                    cp("dve", mkT[:, :, s * 128:(s + 1) * 128], pt[:, 0:512].rearrange("p (h t) -> p h t", h=4), [ptk], ["mkT"])
                for s in range(2):
                    pipe(mk_item(s))
                flush()
                wt, wk = wget("mv")
                for s in range(2):
                    z, zk = tok_section(wt, wk, s)
                    vo, vok = f32w.next()
                    cp("act", vo[:], z[:, :], [zk], [vok])
                    dma("pool", mv_p[b, s * 128:(s + 1) * 128, :], vo[:], [vok], [], "st")
                    cp("dve", mvt[:, s, :], vo[:], [vok], ["mvt"])
            if sample or p == 0:
                dma("sp", gb[:], g_norm.partition_broadcast(128), [], ["gb"], "ld")

            xsrc = x_s if sample else x_p
            if not skip_x:
                xphase(lambda s: rows(xsrc, s), ns, nrows, 0)

            zf, zfk = psum("z")
            for s in range(ns):
                for k in range(8):
                    mm(zf[:, s * 8:(s + 1) * 8], hT[:, k, s * 128:(s + 1) * 128], wff[:, k, :], k == 0, k == 7,
                       ["hT%d" % s, "c9"], [zfk])
            lp, lpk = sm.next()
            tt("dve", lp[:, 0:ns * 8].rearrange("p (s h) -> p s h", s=ns), zf[:, 0:ns * 8].rearrange("p (s h) -> p s h", s=ns),
               bf_b[:, :].unsqueeze(1).broadcast_to([128, ns, 8]), ALU.add, [zfk], [lpk])
            le, lek = sm.next()
            act(le[:, 0:ns * 8], lp[:, 0:ns * 8], AF.Exp, [lpk], [lek], scale=-1.0)
            ll, llk = sm.next()
            act(ll[:, 0:ns * 8], le[:, 0:ns * 8], AF.Ln, [lek], [llk], bias=ones_f[:, 0:1])
            lslot0 = 16 if sample else 0
            ts("dve", logf[:, lslot0:lslot0 + ns, :], ll[:, 0:ns * 8].rearrange("p (s h) -> p s h", s=ns), -1.0, None, ALU.mult, None,
               [llk], ["lf%d" % (lslot0 + s) for s in range(ns)])
            if sample:
                dma("pool", lf_s, logf[0:16, 16, :], ["lf16"], [], "st")
            else:
                dma("pool", lf_p[b, p * tp:(p + 1) * tp, :].rearrange("(s q) h -> q s h", q=128), logf[:, 0:ns, :],
                    ["lf%d" % s for s in range(ns)], [], "st")
            for s in range(ns):
                c, ckey = cumsum_block(kb0 + s, lslot0 + s)
                cp("dve", cc_[:, s, :], c[:, 0:8], [ckey], ["cc%d" % s])
                cp("dve", cparts[:, s, :, 0], cc_[:, s, :], ["cc%d" % s], ["cp%d" % s])
                r1, r1k = sm2.next()
                tt("dve", r1[:, 0:8], cc_[:, s, :], cparts[:, s, :, 0], ALU.subtract, ["cc%d" % s, "cp%d" % s], [r1k])
                cp("dve", cparts[:, s, :, 1], r1[:, 0:8], [r1k], ["cp%d" % s])
                r2, r2k = sm2.next()
                tt("dve", r2[:, 0:8], r1[:, 0:8], cparts[:, s, :, 1], ALU.subtract, [r1k, "cp%d" % s], [r2k])
                cp("dve", cparts[:, s, :, 2], r2[:, 0:8], [r2k], ["cp%d" % s])

            wt, wk = wget("fq")

            def fq_item(s, wt=wt, wk=wk):
                z, zk = tok_section(wt, wk, s)
                r, rk = head_rstd(z, zk, 8, 64)
                yield
                yield
                qa, qak = qat.next()
                tt("dve", qa[:, :, 0:64], z[:, :].rearrange("p (h d) -> p h d", h=8),
                   r[:, 0:8].unsqueeze(2).broadcast_to([128, 8, 64]), ALU.mult, [zk, rk], [qak])
                cp("dve", qa[:, :, 64:67], cparts[:, s, :, :], ["cp%d" % s], [qak])
                yield
                yield
                pt, ptk = psum("t")
                for h in range(8):
                    tp_(pt[0:67, h * 128:(h + 1) * 128], qa[:, h, 0:67], [qak], [ptk])
                ts("dve", qT_aug[0:67, :, s * 128:(s + 1) * 128], pt[0:67, :].rearrange("p (h t) -> p h t", h=8),
                   gq67[0:67, 0:1], None, ALU.mult, None, [ptk, "c6"], ["qT%d" % s])
            wt, wk = wget("fk")

            def fk_item(s, wt=wt, wk=wk):
                z, zk = tok_section(wt, wk, s)
                r, rk = head_rstd(z, zk, 8, 64)
                yield
                yield
                kn, knk = f32w.next()
                tt("dve", kn[:].rearrange("p (h d) -> p h d", h=8), z[:, :].rearrange("p (h d) -> p h d", h=8),
                   r[:, 0:8].unsqueeze(2).broadcast_to([128, 8, 64]), ALU.mult, [zk, rk], [knk])
                ko, kok = f32w.next()
                tt("dve", ko[:].rearrange("p (h d) -> p h d", h=8), kn[:].rearrange("p (h d) -> p h d", h=8),
                   gk_b[:, :].unsqueeze(1).broadcast_to([128, 8, 64]), ALU.mult, [knk], [kok])
                dma("pool", rows(fk_s if sample else fk_p, s), ko[0:nrows, :], [kok], [], "st")
                yield from k_side(ko[:], kok, kb0 + s)
            for s in range(ns):
                pipe(fq_item(s))
                pipe(fk_item(s))
            wt, wk = wget("fv")

            def fv_body(s, wt=wt, wk=wk):
                z, zk = tok_section(wt, wk, s)
                vo, vok = f32w.next()
                cp("act", vo[:], z[:, :], [zk], [vok])
                dma("pool", rows(fv_s if sample else fv_p, s), vo[0:nrows, :], [vok], [], "st")
                v_side(vo[:], vok, kb0 + s)
            for s in range(ns):
                pipe(one(lambda s=s: fv_body(s)))
            wt, wk = wget("fg")
            for cc in range(4):
                pipe(one(lambda cc=cc, wt=wt, wk=wk: feat_gate(wt, wk, cc, 0, n, foxg[:, cc, 0:n], "foxg%d" % cc)))
            flush()
            qb0 = kb0
            nq = ns
            nkb = qb0 + nq
            def att_item(h, kb, ot, otk):
                d = kb - qb0
                c0 = max(d, 0) * 128
                st_, stk = psum("s")
                mm(st_[:, c0:n], kT_aug[0:67, h, kb * 128:(kb + 1) * 128], qT_aug[0:67, h, c0:n], True, d < 0,
                   ["kT%d" % kb] + ["qT%d" % i for i in range(c0 // 128, nq)], [stk])
                if d >= 0:
                    mm(st_[:, c0:c0 + 128], ident_bf[:], maskneg_bf[:], False, True, [], [stk])
                pw, pwk = bfw.next()
                act(pw[:, c0:n], st_[:, c0:n], AF.Exp, [stk, "negc%d" % kb], [pwk], bias=negc[:, kb, h:h + 1])
                pr = (h // 2) * 192 + (64 if h % 2 else 0)
                yield
                yield
                mm(ot[:, c0:n], Vt[:, kb, pr:pr + 128], pw[:, c0:n], kb == 0, kb == nkb - 1, ["V%d" % kb, pwk], [otk])
                if kb == nkb - 1:
                    nb, db = (64, 0) if h % 2 else (0, 64)
                    rec, reck = f32w.next()
                    P.add("dve", lambda e: e.reciprocal(out=rec[db:db + 64, 0:n], in_=ot[db:db + 64, 0:n]), [otk], [reck])
                    tm, tmk = f32w.next()
                    tt("dve", tm[nb:nb + 64, 0:n], ot[nb:nb + 64, 0:n], rec[db:db + 64, 0:n], ALU.mult, [otk, reck], [tmk])
                    fkey = "foxg%d" % (h // 2)
                    tt("dve", foxg[nb:nb + 64, h // 2, 0:n], tm[nb:nb + 64, 0:n], foxg[nb:nb + 64, h // 2, 0:n], ALU.mult,
                       [tmk, fkey], [fkey])
            for h in range(8):
                ot, otk = psum("o")
                for kb in range(nkb):
                    pipe(att_item(h, kb, ot, otk))

            rtabs = {}
            rope_w = {"rq": wget("rq"), "rk": wget("rk")}
            if True:
                def rope_item(s, which, dstT, toff):
                    wt, wk = rope_w[which]
                    if which == "rq":
                        rtt, rtk = rt_next()
                        if sample:
                            dma("sp", rtt, rope_s, [], [rtk], "ld")
                        else:
                            dma("sp", rtt, rope_p[p * tp + s * 128:p * tp + (s + 1) * 128, :], [], [rtk], "ld")
                        rtabs[s] = (rtt, rtk)
                    rtt, rtk = rtabs[s]
                    tab = rtt.rearrange("p (a h d) -> p a h d", a=4, h=4)
                    Ct = tab[:, toff, :, :]
                    St = tab[:, toff + 1, :, :]
                    z, zk = tok_section(wt, wk, s)
                    z4 = z[:, :].rearrange("p (h t d) -> p h t d", h=4, t=2)
                    yield
                    yield
                    if which == "rq":
                        t1, t1k = bfw.next()
                        t14 = t1[:, :].rearrange("p (h t d) -> p h t d", h=4, t=2)
                        tt("dve", t14, z4, Ct.unsqueeze(2).broadcast_to([128, 4, 2, 64]), ALU.mult, [zk, rtk], [t1k])
                        t2, t2k = bfw.next()
                        t24 = t2[:, :].rearrange("p (h t d) -> p h t d", h=4, t=2)
                        stt("dve", t24[:, :, 0, :], z4[:, :, 1, :], -1.0, St, ALU.mult, ALU.mult, [zk, rtk], [t2k])
                        tt("dve", t24[:, :, 1, :], z4[:, :, 0, :], St, ALU.mult, [zk, rtk, t2k], [t2k])
                        yield
                        yield
                        pq, pqk = psum("z")
                        for h in range(4):
                            mm(pq[:, h * 128:(h + 1) * 128], t1[:, h * 128:(h + 1) * 128], ident_bf[:], True, False, [t1k], [pqk])
                            mm(pq[:, h * 128:(h + 1) * 128], t2[:, h * 128:(h + 1) * 128], ident_bf[:], False, True, [t2k], [pqk])
                        cp("act", dstT[:, :, s * 128:(s + 1) * 128], pq[:, :].rearrange("p (h t) -> p h t", h=4), [pqk],
                           ["%sT%d" % (which, s)])
                        return
                    t1, t1k = f32w.next()
                    t14 = t1[:, :].rearrange("p (h t d) -> p h t d", h=4, t=2)
                    tt("dve", t14, z4, Ct.unsqueeze(2).broadcast_to([128, 4, 2, 64]), ALU.mult, [zk, rtk], [t1k])
                    t2, t2k = f32w.next()
                    t24 = t2[:, :].rearrange("p (h t d) -> p h t d", h=4, t=2)
                    tt("dve", t24[:, :, 0, :], z4[:, :, 1, :], St, ALU.mult, [zk, rtk], [t2k])
                    tt("dve", t24[:, :, 1, :], z4[:, :, 0, :], St, ALU.mult, [zk, rtk, t2k], [t2k])
                    if which == "rq":
                        rb, rbk = bfw.next()
                        rb_ap = rb[:, :]
                    else:
                        rb_ap = rk_tok[:, s, :]
                        rbk = "rk_tok%d" % s
                    rb4 = rb_ap.rearrange("p (h t d) -> p h t d", h=4, t=2)
                    tt("pool", rb4[:, :, 0, :], t14[:, :, 0, :], t24[:, :, 0, :], ALU.subtract, [t1k, t2k], [rbk])
                    tt("pool", rb4[:, :, 1, :], t14[:, :, 1, :], t24[:, :, 1, :], ALU.add, [t1k, t2k, rbk], [rbk])
                    yield
                    yield
                    pt, ptk = psum("t")
                    for h in range(4):
                        tp_(pt[:, h * 128:(h + 1) * 128], rb_ap[:, h * 128:(h + 1) * 128], [rbk], [ptk])
                    cp("act", dstT[:, :, s * 128:(s + 1) * 128], pt[:, 0:512].rearrange("p (h t) -> p h t", h=4), [ptk],
                       ["%sT%d" % (which, s)])
                for s in range(ns):
                    pipe(rope_item(s, "rq", rqT, 0))
                    pipe(rope_item(s, "rk", rkT, 2))
            for j in range(2):
                wt, wk = wget("rv%d" % j)

                def rv_body(s, j=j, wt=wt, wk=wk):
                    z, zk = tok_section(wt, wk, s)
                    cp("act", rvt[:, s, j * 512:(j + 1) * 512], z[:, :], [zk], ["rv%d_%d" % (s, j)])
                for s in range(ns):
                    pipe(one(lambda s=s, f=rv_body: f(s)))
            for j in range(2):
                wt, wk = wget("rg%d" % j)
                for cc in range(4):
                    pipe(one(lambda cc=cc, j=j, wt=wt, wk=wk: feat_gate(wt, wk, cc, 0, n, retg[:, j * 4 + cc, 0:n], "retg%d" % (j * 4 + cc))))
            flush()

            pend_on = []

            def ret_item(s, h):
                cs = slice(s * 128, (s + 1) * 128)
                pp, ppk = psum("o")
                mm(pp[:, 0:128], rkT[:, h, cs], rqT[:, h, cs], True, True, ["rkT%d" % s, "rqT%d" % s], [ppk])
                pm, pmk = bfw.next()
                stt("dve", pm[:, 0:128], pp[:, 0:128], 1.0 / gL[h], mask01_f[:], ALU.mult, ALU.mult, [ppk], [pmk])
                yield
                while pend_on:
                    pend_on.pop(0)()
                orr, ork = psum("s")
                vk_ = "rv%d_%d" % (s, h // 2)
                mm(orr[:, 0:256], pm[:, 0:128], rvt[:, s, h * 256:(h + 1) * 256], True, False, [pmk, vk_], [ork])
                mm(orr[:, 0:256], rqT[:, h, cs], S_bf[:, h, :], False, True, ["rqT%d" % s, "S_bf%d" % h], [ork])
                su, suk = psum("o")
                mm(su[:, 0:256], rk_tok[:, s, h * 128:(h + 1) * 128], rvt[:, s, h * 256:(h + 1) * 256], True, True,
                   ["rk_tok%d" % s, vk_], [suk])
                stt("dve", S_f[:, h, :], S_f[:, h, :], gL[h], su[:, 0:256], ALU.mult, ALU.add, [suk, "S_f%d" % h], ["S_f%d" % h])
                cp("act", S_bf[:, h, :], S_f[:, h, :], ["S_f%d" % h], ["S_bf%d" % h])
                bs, bsk = sm2.next()
                P.add("dve", lambda e: e.bn_stats(out=bs[:, 0:6], in_=orr[:, 0:256]), [ork], [bsk])
                ba, bak = sm2.next()
                P.add("dve", lambda e: e.bn_aggr(out=ba[:, 0:2], in_=bs[:, 0:6]), [bsk], [bak])
                v_, vk_2 = sm2.next()
                tt("pool", v_[:, 0:1], ba[:, 1:2], epst[:, 0:1], ALU.add, [bak], [vk_2])
                r, rk = sm2.next()
                tt("pool", r[:, 0:1], v_[:, 0:1], mhalf[:, 0:1], ALU.pow, [vk_2], [rk])
                nb_, nbk = sm2.next()
                tt("pool", nb_[:, 0:1], ba[:, 0:1], r[:, 0:1], ALU.mult, [bak, rk], [nbk])
                tt("pool", nb_[:, 0:1], nb_[:, 0:1], mone[:, 0:1], ALU.mult, [nbk], [nbk])
                on, onk = bfw.next()

                def apply_gn():
                    act(on[:, 0:256], orr[:, 0:256], AF.Identity, [ork, rk, nbk], [onk], bias=nb_[:, 0:1], scale=r[:, 0:1])
                pend_on.append(apply_gn)
                yield
                if apply_gn in pend_on:
                    pend_on.remove(apply_gn)
                    apply_gn()
                yield
                pt, ptk = psum("t")
                for j in range(2):
                    tp_(pt[:, j * 128:(j + 1) * 128], on[:, j * 128:(j + 1) * 128], [onk], [ptk])
                gk_ = ["retg%d" % (h * 2), "retg%d" % (h * 2 + 1)]
                tt("dve", retg[:, h * 2:h * 2 + 2, cs], pt[:, 0:256].rearrange("p (j t) -> p j t", j=2),
                   retg[:, h * 2:h * 2 + 2, cs], ALU.mult, [ptk] + gk_, gk_)
            for s in range(ns):
                for h in range(4):
                    pipe(ret_item(s, h))
            flush()
            if sample:
                dma("pool", sr_s.rearrange("h k v -> k h v"), S_f[:], SFK, [], "st")
            elif p == SEQ // tp - 1:
                dma("pool", sr_p[b].rearrange("h k v -> k h v"), S_f[:], SFK, [], "st")

            wt, wk = wget("mq")

            def mq_item(s, wt=wt, wk=wk):
                z, zk = tok_section(wt, wk, s)
                r, rk = head_rstd(z, zk, 4, 128)
                yield
                mqa, mqak = bfw.next()
                tt("dve", mqa[:, :].rearrange("p (h d) -> p h d", h=4), z[:, :].rearrange("p (h d) -> p h d", h=4),
                   r[:, 0:4].unsqueeze(2).broadcast_to([128, 4, 128]), ALU.mult, [zk, rk], [mqak])
                yield
                pt, ptk = psum("t")
                for h in range(4):
                    tp_(pt[:, h * 128:(h + 1) * 128], mqa[:, h * 128:(h + 1) * 128], [mqak], [ptk])
                act(mqT[:, :, s * 128:(s + 1) * 128], pt[:, 0:512].rearrange("p (h t) -> p h t", h=4), AF.Copy,
                    [ptk, "c7"], ["mqT%d" % s], scale=gmq_s[:, 0:1])
            for s in range(ns):
                pipe(mq_item(s))
            wt, wk = wget("mg")
            for cc in range(4):
                pipe(one(lambda cc=cc, wt=wt, wk=wk: feat_gate(wt, wk, cc, 0, n, memg[:, cc, 0:n], "memg%d" % cc)))
            flush()
            def mem_item(h, mb, om, omk, dm, dmk):
                st_, stk = psum("s")
                mm(st_[:, 0:n], mkT[:, h, mb * 128:(mb + 1) * 128], mqT[:, h, 0:n], True, True,
                   ["mkT"] + ["mqT%d" % i for i in range(ns)], [stk])
                pw, pwk = bfw.next()
                act(pw[:, 0:n], st_[:, 0:n], AF.Exp, [stk], [pwk])
                yield
                yield
                mm(om[:, 0:n], mvt[:, mb, h * 128:(h + 1) * 128], pw[:, 0:n], mb == 0, mb == 1, ["mvt", pwk], [omk])
                mm(dm[:, 0:n], ones_bf[:], pw[:, 0:n], mb == 0, mb == 1, [pwk], [dmk])
                if mb == 1:
                    rec, reck = f32w.next()
                    P.add("dve", lambda e: e.reciprocal(out=rec[:, 0:n], in_=dm[:, 0:n]), [dmk], [reck])
                    tm, tmk = f32w.next()
                    tt("dve", tm[:, 0:n], om[:, 0:n], rec[:, 0:n], ALU.mult, [omk, reck], [tmk])
                    tt("dve", memg[:, h, 0:n], tm[:, 0:n], memg[:, h, 0:n], ALU.mult, [tmk, "memg%d" % h], ["memg%d" % h])
            for h in range(4):
                om, omk = psum("o")
                dm, dmk = psum("s")
                for mb in range(2):
                    pipe(mem_item(h, mb, om, omk, dm, dmk))

            flush()
            hk_all = hkeys(0, n)
            qk_all = ["qT%d" % i for i in range(ns)]
            pre_x, xl = [], []
            if prestage_next:
                def nrows_fn(s2):
                    t0 = (p + 1) * tp + s2 * 128
                    return x_p[b, t0:t0 + 128, :]
                xall = x_items(nrows_fn, NSP, 128, 0, [1, 1, 2, 2])
                pre_x, xl = xall[:2], xall[2:]
                for g_ in pre_x:
                    next(g_)
            for j in range(2):
                for bi, (gname, pname, src, nk, skeys) in enumerate((
                        ("g0%d" % j, "pf%d" % j, foxg, 4, ["foxg%d" % i for i in range(4)]),
                        ("g1%d" % j, "pr%d" % j, retg, 8, ["retg%d" % i for i in range(8)]),
                        ("g2%d" % j, "pm%d" % j, memg, 4, ["memg%d" % i for i in range(4)]))):
                    gt, gtk = wget(gname)
                    pt_, ptk_ = wget(pname)
                    if j == 0 and bi == 1:
                        for g_ in pre_x:
                            next(g_)
                    for cc in range(4):
                        g, gk = psum("z")
                        for k in range(8):
                            mm(g[:, 0:n], gt[:, k, cc * 128:(cc + 1) * 128], hT[:, k, 0:n], k == 0, k == 7, hk_all + [gtk], [gk])
                        bp, bpk = psum("s")
                        for k in range(nk):
                            mm(bp[:, 0:n], pt_[:, k, cc * 128:(cc + 1) * 128], src[:, k, 0:n], k == 0, k == nk - 1, skeys + [ptk_], [bpk])
                        th, thk = bfw.next()
                        col = bi * 8 + j * 4 + cc
                        act(th[:, 0:n], g[:, 0:n], AF.Tanh, [gk, "c8"], [thk], scale=0.5, bias=bm_half[:, col:col + 1])
                        ak = "acc%d" % cc
                        rtk_ = "rt%d" % (cc // 2)
                        if bi == 0:
                            stt("dve", acc[:, cc, 0:n], th[:, 0:n], 1.0, bp[:, 0:n], ALU.add, ALU.mult, [thk, bpk], [ak, rtk_])
                        else:
                            t_, tk_ = f32w.next()
                            stt("dve", t_[:, 0:n], th[:, 0:n], 1.0, bp[:, 0:n], ALU.add, ALU.mult, [thk, bpk], [tk_])
                            if bi == 1:
                                tt("dve", acc[:, cc, 0:n], acc[:, cc, 0:n], t_[:, 0:n], ALU.add, [ak, tk_, rtk_], [ak])
                            else:
                                tt("dve", mrgT[:, j * 4 + cc, 0:n], acc[:, cc, 0:n], t_[:, 0:n], ALU.add, [ak, tk_, rtk_],
                                   ["mrg%d" % (j * 4 + cc)] + qk_all)
            mk_all = ["mrg%d" % i for i in range(8)]
            wo = [wget("wo0"), wget("wo1")]
            def y_item(s, j):
                xh, xhk = f32w.next()
                dma("sp", xh[0:nrows, :], rows(xsrc, s)[:, j * 512:(j + 1) * 512], [], [xhk], "ld")
                y, yk = psum("s")
                for k in range(8):
                    mm(y[:, :], mrgT[:, k, s * 128:(s + 1) * 128], wo[j][0][:, k, :], k == 0, k == 7, mk_all + qk_all + [wo[j][1]], [yk])
                yo, yok = f32w.next()
                stt("dve", yo[:], y[:, :], 0.25, xh[:, :], ALU.mult, ALU.add, [yk, xhk], [yok])
                ydst = rows(y_s if sample else y_p, s)
                dma("pool", ydst[:, j * 512:(j + 1) * 512], yo[0:nrows, :], [yok], [], "st")
                return
                yield
            yl = [y_item(s, j) for s in range(ns) for j in range(2)]
            active.extend(pre_x)
            for i in range(max(len(yl), len(xl))):
                if i < len(yl):
                    pipe(yl[i])
                if i < len(xl):
                    pipe(xl[i])
            flush()

        for i, (kind, b, p) in enumerate(passes):
            skip_x = i > 0 and passes[i - 1][0] == "p" and kind == "p" and passes[i - 1][1] == b and passes[i - 1][2] == p - 1
            nxt = passes[i + 1] if i + 1 < len(passes) else None
            pre = kind == "p" and nxt is not None and nxt[0] == "p" and nxt[1] == b and nxt[2] == p + 1
            run_pass(kind, b, p, skip_x, pre)
        assert wpos[0] == len(plan)
        P.barrier(engs=("sp",))
        print("sbuf bytes remaining", nc.sbuf_bytes_remaining)
        P.emit()
    return nc


def _consts():
    ident = np.eye(128, dtype=np.float32)
    triu = np.triu(np.ones((128, 128), np.float32))
    maskneg = np.where(triu > 0, 0.0, -30000.0).astype(np.float32)
    mask01 = triu.copy()

    def rope_tab(pos, idx, L):
        inv = 10000.0 ** (-np.arange(64, dtype=np.float64) / 64.0)
        ang = pos.astype(np.float64)[:, None] * inv[None, :]
        cos, sin = np.cos(ang), np.sin(ang)
        tab = np.zeros((len(pos), 4, 4, 64), np.float64)
        for h in range(4):
            g = 1.0 - 2.0 ** (-5 - h)
            dq = g ** (idx + 1.0)
            dk = g ** (-(idx + 1.0)) * 128.0 ** -0.5 * g ** L
            tab[:, 0, h] = cos * dq[:, None]
            tab[:, 1, h] = sin * dq[:, None]
            tab[:, 2, h] = cos * dk[:, None]
            tab[:, 3, h] = sin * dk[:, None]
        return tab.reshape(len(pos), 1024).astype(np.float32)

    pos_p = np.arange(SEQ)
    rope_p = rope_tab(pos_p, (pos_p % 128).astype(np.float64), 128)
    rope_s = np.zeros((128, 1024), np.float32)
    rope_s[:16] = rope_tab(SEQ + np.arange(16), np.arange(16, dtype=np.float64), 16)
    return dict(c_ident=ident, c_triu=triu, c_maskneg=maskneg, c_mask01=mask01, rope_p=rope_p, rope_s=rope_s)


_NC_CACHE = {}


def kernel(x_prompt, x_sample, mem_prompt, cache_fox_k, cache_fox_v, cache_fox_logf, state_ret, cache_mem_k, cache_mem_v,
           g_norm, g_mem_norm, w_in, b_f, b_merge, g_fox_q, g_fox_k, g_mem_q, g_mem_k, w_mem_kv,
           w_p_fox, w_p_ret, w_p_mem, w_out):
    f = lambda a: np.ascontiguousarray(np.asarray(a, dtype=np.float32))
    x_prompt, x_sample, mem_prompt = f(x_prompt), f(x_sample), f(mem_prompt)
    cache_fox_k, cache_fox_v, cache_fox_logf = f(cache_fox_k), f(cache_fox_v), f(cache_fox_logf)
    state_ret, cache_mem_k, cache_mem_v = f(state_ret), f(cache_mem_k), f(cache_mem_v)
    if "nc" not in _NC_CACHE:
        _NC_CACHE["nc"] = build_program()
    nc = _NC_CACHE["nc"]
    shared = dict(g_norm=f(g_norm)[0], g_mem_norm=f(g_mem_norm)[0], w_in=f(w_in)[0], b_f=f(b_f)[0], b_merge=f(b_merge)[0],
                  g_fox_q=f(g_fox_q)[0], g_fox_k=f(g_fox_k)[0], g_mem_q=f(g_mem_q)[0], g_mem_k=f(g_mem_k)[0],
                  w_mem_kv=f(w_mem_kv)[0], w_p_fox=f(w_p_fox)[0], w_p_ret=f(w_p_ret)[0], w_p_mem=f(w_p_mem)[0],
                  w_out=f(w_out)[0])
    shared.update(_consts())
    in_maps = []
    for c in range(NCORES):
        m = dict(shared)
        m.update(x_p=x_prompt[2 * c:2 * c + 2], x_s=x_sample[c], mem_p=mem_prompt[2 * c:2 * c + 2],
                 ck=np.ascontiguousarray(cache_fox_k[0, c].reshape(SEQ, 512)),
                 cv=np.ascontiguousarray(cache_fox_v[0, c].reshape(SEQ, 512)),
                 clf=np.ascontiguousarray(cache_fox_logf[0, c]),
                 sret=np.ascontiguousarray(state_ret[0, c]),
                 cmk=np.ascontiguousarray(cache_mem_k[0, c].reshape(256, 512)),
                 cmv=np.ascontiguousarray(cache_mem_v[0, c].reshape(256, 512)))
        in_maps.append(m)
    res = run_bass_kernel_spmd(nc, in_maps, core_ids=list(range(NCORES)))
    R = res.results
    cat = lambda k: np.concatenate([np.asarray(r[k]) for r in R], axis=0)
    stk = lambda k: np.stack([np.asarray(r[k]) for r in R], axis=0)
    y_p = cat("y_p")
    y_s = stk("y_s")
    fk_p = cat("fk_p").reshape(1, 16, SEQ, 8, 64)
    fv_p = cat("fv_p").reshape(1, 16, SEQ, 8, 64)
    lf_p = cat("lf_p").reshape(1, 16, SEQ, 8)
    sr_p = cat("sr_p").reshape(1, 16, 4, 128, 256)
    mk_p = cat("mk_p").reshape(1, 16, 256, 4, 128)
    mv_p = cat("mv_p").reshape(1, 16, 256, 4, 128)
    fk_s = stk("fk_s").reshape(1, 8, 16, 8, 64)
    fv_s = stk("fv_s").reshape(1, 8, 16, 8, 64)
    lf_s = stk("lf_s").reshape(1, 8, 16, 8)
    sr_s = stk("sr_s").reshape(1, 8, 4, 128, 256)
    return (y_p.astype(np.float32), y_s.astype(np.float32), fk_p, fv_p, lf_p, sr_p, mk_p, mv_p, fk_s, fv_s, lf_s, sr_s)
```
